# Optimizing a Trainium2 kernel written in Bass

```python
import jax
import jax.numpy as jnp
from jax import lax
import numpy as np

D_MODEL = 1024
BATCH = 2
SEQ = 16384
DEPTH = 2

GRID_W = 64
CTX_LEN = 256
EPS = 1e-6
NEG_INF = -1e30

HEAD_DIM = 64
N_HEADS = (D_MODEL // 2) // HEAD_DIM
KV_HEADS = N_HEADS // 4
GROUP = N_HEADS // KV_HEADS
WINDOW = 128
ATTN_BLOCK = 128
ROPE_BASE = 10000.0
ROPE_FREQS = HEAD_DIM // 4

LRU_WIDTH = D_MODEL // 4
LRU_BLOCKS = 4
LRU_BLOCK_W = LRU_WIDTH // LRU_BLOCKS
LRU_C = 8.0
CONV_W = 4
CONV_PAD = (CONV_W // 2, CONV_W - 1 - CONV_W // 2)
SQRT_FLOOR = 1e-12

HG_HEADS = 4
HG_DK = (D_MODEL // 4) // HG_HEADS
HG_DV = (D_MODEL // 4) // HG_HEADS
HG_CHUNK = 32

ATTN_Q_W = N_HEADS * HEAD_DIM
ATTN_KV_W = KV_HEADS * HEAD_DIM
HG_K_W = HG_HEADS * HG_DK
HG_V_W = HG_HEADS * HG_DV
MIX_W = ATTN_Q_W + LRU_WIDTH + HG_V_W
IN_SPLITS = (ATTN_Q_W, ATTN_KV_W, ATTN_KV_W, LRU_WIDTH, LRU_WIDTH, HG_K_W, HG_K_W, HG_K_W, HG_V_W, HG_V_W)
IN_W = ATTN_Q_W + 2 * ATTN_KV_W + 2 * LRU_WIDTH + 3 * HG_K_W + 2 * HG_V_W

N_EXPERTS = 32
TOP_K = 4
D_FF = D_MODEL
SWIGLU_LIMIT = 7.0
SWIGLU_ALPHA = 1.702
MOE_BLOCK = 128

kernel_name = 'hybrid_dit_swa_rglru_hgrn2_moe'


def rms_norm(x, g):
    xf = x.astype(jnp.float32)
    y = xf * lax.rsqrt(jnp.mean(xf * xf, axis=-1, keepdims=True) + EPS)
    return (y * g.astype(jnp.float32)).astype(x.dtype)


def modulate(h, shift, scale):
    return h * (1.0 + scale) + shift


def axial_rope_tables(n_tokens, dtype):
    n_rows = n_tokens // GRID_W
    rows = jnp.repeat(jnp.arange(n_rows, dtype=jnp.float32), GRID_W)
    cols = jnp.tile(jnp.arange(GRID_W, dtype=jnp.float32), n_rows)
    inv_freq = ROPE_BASE ** (-jnp.arange(ROPE_FREQS, dtype=jnp.float32) / ROPE_FREQS)
    ang = jnp.stack([rows[:, None] * inv_freq, cols[:, None] * inv_freq], axis=1)
    return jnp.cos(ang).astype(dtype), jnp.sin(ang).astype(dtype)


def apply_axial_rope(x, cos, sin):
    b, t, h, _ = x.shape
    xr = x.reshape(b, t, h, 2, 2, ROPE_FREQS)
    x1, x2 = xr[..., 0, :], xr[..., 1, :]
    cs, sn = cos[:, None], sin[:, None]
    out = jnp.stack([x1 * cs - x2 * sn, x1 * sn + x2 * cs], axis=-2)
    return out.reshape(b, t, h, HEAD_DIM)


def sink_softmax(parts, sink):
    snk = jnp.broadcast_to(sink[None, :, :, None, None], parts[0].shape[:-1] + (1,))
    p = jax.nn.softmax(jnp.concatenate(parts + [snk], axis=-1), axis=-1)
    return p[..., :-1]


def window_attention(q_l, k_l, v_l, q_c, k_c, v_c, sink, cos, sin, with_ctx_out):
    bsz, seq, _ = q_l.shape
    ctx_len = k_c.shape[1]
    nb = seq // ATTN_BLOCK
    scale = HEAD_DIM ** -0.5

    def heads(t, n):
        return t.reshape(t.shape[0], t.shape[1], n, HEAD_DIM)

    ql = apply_axial_rope(heads(q_l, N_HEADS), cos, sin) * scale
    kl = apply_axial_rope(heads(k_l, KV_HEADS), cos, sin)
    vl = heads(v_l, KV_HEADS)
    kc = heads(k_c, KV_HEADS)
    vc = heads(v_c, KV_HEADS)
    snk = sink.astype(jnp.float32).reshape(KV_HEADS, GROUP)

    def band(t):
        tp = jnp.pad(t, ((0, 0), (ATTN_BLOCK, ATTN_BLOCK), (0, 0), (0, 0)))
        tp = tp.reshape(bsz, nb + 2, ATTN_BLOCK, KV_HEADS, HEAD_DIM)
        return jnp.concatenate([tp[:, :-2], tp[:, 1:-1], tp[:, 2:]], axis=2)

    start = jnp.arange(nb)[:, None, None] * ATTN_BLOCK
    qpos = start + jnp.arange(ATTN_BLOCK)[None, :, None]
    kpos = start - ATTN_BLOCK + jnp.arange(3 * ATTN_BLOCK)[None, None, :]
    valid = (jnp.abs(qpos - kpos) <= WINDOW) & (kpos >= 0) & (kpos < seq)
    n_loc = 3 * ATTN_BLOCK

    def block(args):
        qb, kb, vb, mb = args
        s_loc = jnp.where(mb, jnp.einsum('bqhgd,bkhd->bhgqk', qb, kb).astype(jnp.float32), NEG_INF)
        s_ctx = jnp.einsum('bqhgd,bchd->bhgqc', qb, kc).astype(jnp.float32)
        p = sink_softmax([s_loc, s_ctx], snk).astype(vb.dtype)
        return (jnp.einsum('bhgqk,bkhd->bqhgd', p[..., :n_loc], vb)
                + jnp.einsum('bhgqc,bchd->bqhgd', p[..., n_loc:], vc))

    qb = jnp.moveaxis(ql.reshape(bsz, nb, ATTN_BLOCK, KV_HEADS, GROUP, HEAD_DIM), 1, 0)
    o = lax.map(block, (qb, jnp.moveaxis(band(kl), 1, 0), jnp.moveaxis(band(vl), 1, 0), valid))
    out_l = jnp.moveaxis(o, 0, 1).reshape(bsz, seq, ATTN_Q_W)
    out_c = None
    if with_ctx_out:
        qc = heads(q_c, N_HEADS).reshape(bsz, ctx_len, KV_HEADS, GROUP, HEAD_DIM) * scale
        s = jnp.einsum('bqhgd,bkhd->bhgqk', qc, kc).astype(jnp.float32)
        p = sink_softmax([s], snk).astype(vc.dtype)
        out_c = jnp.einsum('bhgqk,bkhd->bqhgd', p, vc).reshape(bsz, ctx_len, ATTN_Q_W)
    return out_l, out_c


def short_conv(x, w, b):
    y = lax.conv_general_dilated(x, w[:, None, :].astype(x.dtype), window_strides=(1,),
                                 padding=[CONV_PAD], dimension_numbers=('NWC', 'WIO', 'NWC'),
                                 feature_group_count=x.shape[-1])
    return y + b


def linear_scan(a, b, h0):
    def combine(l, r):
        return l[0] * r[0], r[0] * l[1] + r[1]
    a_cum, h = lax.associative_scan(combine, (a, b), axis=1)
    h = h + a_cum * h0[:, None, :]
    return h, h[:, -1]


def prefix_scan(scan_fn, ctx_args, lat_args, init, reverse):
    flip = (lambda t: jnp.flip(t, axis=1)) if reverse else (lambda t: t)
    o_c, s_c = scan_fn(*[flip(t) for t in ctx_args], init)
    o_l, _ = scan_fn(*[flip(t) for t in lat_args], s_c)
    return flip(o_c), flip(o_l)


def rglru_gates(u, wr, br, wi, bi, lam):
    bsz, t, ch = u.shape
    ub = u.reshape(bsz, t, LRU_BLOCKS, LRU_BLOCK_W)
    r = jax.nn.sigmoid(jnp.einsum('btnc,ncd->btnd', ub, wr).reshape(bsz, t, ch) + br)
    i = jax.nn.sigmoid(jnp.einsum('btnc,ncd->btnd', ub, wi).reshape(bsz, t, ch) + bi)
    log_a = -LRU_C * r.astype(jnp.float32) * jax.nn.softplus(-lam.astype(jnp.float32))
    mult = jnp.sqrt(jnp.maximum(-jnp.expm1(2.0 * log_a), SQRT_FLOOR))
    b = mult * (i * u).astype(jnp.float32)
    return jnp.exp(log_a), b


def rglru_mixer(xl, gl, xc, gc, conv_w, conv_b, wr, br, wi, bi, lam, with_ctx_out):
    ul = short_conv(xl, conv_w, conv_b)
    uc = short_conv(xc, conv_w, conv_b)
    h0 = jnp.zeros((xl.shape[0], LRU_WIDTH), jnp.float32)
    outs = [prefix_scan(linear_scan,
                        rglru_gates(uc, wr[d], br[d], wi[d], bi[d], lam[d]),
                        rglru_gates(ul, wr[d], br[d], wi[d], bi[d], lam[d]),
                        h0, reverse=(d == 1)) for d in range(2)]
    yl = (outs[0][1] + outs[1][1]).astype(xl.dtype) * jax.nn.gelu(gl)
    yc = (outs[0][0] + outs[1][0]).astype(xc.dtype) * jax.nn.gelu(gc) if with_ctx_out else None
    return yl, yc


def gla_chunked(q, k, v, log_f, s0):
    bsz, t, h, _ = q.shape
    n = t // HG_CHUNK

    def r(a):
        return a.reshape(bsz, n, HG_CHUNK, h, a.shape[-1])

    q, k, v = r(q).astype(jnp.float32), r(k).astype(jnp.float32), r(v)
    b = jnp.cumsum(r(log_f).astype(jnp.float32), axis=2)
    b_last = b[:, :, -1:]
    b_ref = 0.5 * b_last
    q_i = q * jnp.exp(b - b_ref)
    k_i = k * jnp.exp(b_ref - b)
    q_s = q * jnp.exp(b)
    k_e = k * jnp.exp(b_last - b)
    causal = jnp.tril(jnp.ones((HG_CHUNK, HG_CHUNK), dtype=bool))
    att = jnp.where(causal, jnp.einsum('bnthk,bnshk->bnhts', q_i, k_i), 0.0)
    vf = v.astype(jnp.float32)
    o = jnp.einsum('bnhts,bnshv->bnthv', att, vf)
    kv = jnp.einsum('bnshk,bnshv->nbhkv', k_e, vf)
    decay = jnp.moveaxis(jnp.exp(b_last[:, :, 0]), 1, 0)

    def step(s, xs):
        dcy, kvc = xs
        return dcy[..., None] * s + kvc, s

    s_fin, s_start = lax.scan(step, s0.astype(jnp.float32), (decay, kv))
    o = o + jnp.einsum('bnthk,nbhkv->bnthv', q_s, s_start)
    return o.reshape(bsz, t, h, v.shape[-1]).astype(v.dtype), s_fin


def hgrn2_mixer(pl, pc, lb, norm_g, with_ctx_out):
    def prep(p, d):
        q, zf, zb, v, _ = p
        bsz, t, _ = q.shape
        z = (zf, zb)[d].reshape(bsz, t, HG_HEADS, HG_DK).astype(jnp.float32)
        lbd = lb[d].reshape(HG_HEADS, HG_DK)
        log_f = jnp.log(lbd + (1.0 - lbd) * jax.nn.sigmoid(z))
        k = (1.0 - lbd) * jax.nn.sigmoid(-z)
        return (q.reshape(bsz, t, HG_HEADS, HG_DK), k,
                v.reshape(bsz, t, HG_HEADS, HG_DV), log_f)

    s0 = jnp.zeros((pl[0].shape[0], HG_HEADS, HG_DK, HG_DV), jnp.float32)
    outs = [prefix_scan(gla_chunked, prep(pc, d), prep(pl, d), s0, reverse=(d == 1)) for d in range(2)]

    def readout(o, g):
        bsz, t = g.shape[:2]
        return rms_norm(o, norm_g.reshape(HG_HEADS, HG_DV)).reshape(bsz, t, HG_V_W) * jax.nn.silu(g)

    yl = readout(outs[0][1] + outs[1][1], pl[4])
    yc = readout(outs[0][0] + outs[1][0], pc[4]) if with_ctx_out else None
    return yl, yc


def clamped_swiglu(h, w_gu, b_gu, w_down, b_down):
    gu = h @ w_gu + b_gu
    gate = jnp.minimum(gu[:, :D_FF], SWIGLU_LIMIT)
    up = jnp.clip(gu[:, D_FF:], -SWIGLU_LIMIT, SWIGLU_LIMIT)
    glu = gate * jax.nn.sigmoid(SWIGLU_ALPHA * gate)
    return ((up + 1.0) * glu) @ w_down + b_down


def moe_ffn(h, router_w, router_b, w_gu, b_gu, w_down, b_down):
    n_tok, d = h.shape
    logits = (h @ router_w + router_b).astype(jnp.float32)
    top_val, top_idx = lax.top_k(logits, TOP_K)
    gates = jax.nn.softmax(top_val, axis=-1).astype(h.dtype)
    m = n_tok * TOP_K
    flat_e = top_idx.reshape(m)
    order = jnp.argsort(flat_e)
    sorted_e = flat_e[order]
    sorted_tok = (order // TOP_K).astype(jnp.int32)
    sorted_g = gates.reshape(m)[order]
    counts = jnp.bincount(flat_e, length=N_EXPERTS)
    starts = jnp.cumsum(counts) - counts
    padded = (counts + MOE_BLOCK - 1) // MOE_BLOCK * MOE_BLOCK
    pends = jnp.cumsum(padded)
    pstarts = pends - padded
    dest = pstarts[sorted_e] + jnp.arange(m) - starts[sorted_e]
    n_blocks = -(-m // MOE_BLOCK) + N_EXPERTS
    p_rows = n_blocks * MOE_BLOCK
    tok_buf = jnp.full((p_rows,), n_tok, jnp.int32).at[dest].set(sorted_tok)
    gate_buf = jnp.zeros((p_rows,), h.dtype).at[dest].set(sorted_g)
    blk_e = jnp.minimum(jnp.searchsorted(pends, jnp.arange(n_blocks) * MOE_BLOCK, side='right'), N_EXPERTS - 1)
    h_pad = jnp.concatenate([h, jnp.zeros((1, d), h.dtype)], axis=0)

    def run(args):
        tok, g, e = args
        return clamped_swiglu(h_pad[tok], w_gu[e], b_gu[e], w_down[e], b_down[e]) * g[:, None]

    y = lax.map(run, (tok_buf.reshape(n_blocks, MOE_BLOCK), gate_buf.reshape(n_blocks, MOE_BLOCK), blk_e))
    return jax.ops.segment_sum(y.reshape(p_rows, d), tok_buf, num_segments=n_tok + 1)[:n_tok]


def setup_inputs(seed: int = 0) -> dict:
    key = jax.random.key(seed)
    keys = iter(jax.random.split(key, 32))
    f32 = jnp.float32

    def nrm(shape, scale):
        return jax.random.normal(next(keys), shape, f32) * scale

    def gain(shape):
        return 1.0 + nrm(shape, 0.02)

    L, D = DEPTH, D_MODEL
    a0 = jax.random.uniform(next(keys), (L, 2, LRU_WIDTH), f32, 0.9, 0.999)
    root = a0 ** (1.0 / LRU_C)
    return {
        'x': nrm((BATCH, SEQ, D), 1.0),
        'c': nrm((BATCH, D), 1.0),
        'ctx': nrm((BATCH, CTX_LEN, D), 1.0),
        'c_ctx': nrm((D,), 1.0),
        'ada_w': nrm((L, D, 6 * D), 0.5 * D ** -0.5),
        'ada_b': nrm((L, 6 * D), 0.01),
        'norm1_g': gain((L, D)),
        'w_in': nrm((L, D, IN_W), D ** -0.5),
        'attn_sink': nrm((L, N_HEADS), 0.5),
        'conv_w': nrm((L, CONV_W, LRU_WIDTH), CONV_W ** -0.5),
        'conv_b': nrm((L, LRU_WIDTH), 0.01),
        'lru_wr': nrm((L, 2, LRU_BLOCKS, LRU_BLOCK_W, LRU_BLOCK_W), LRU_BLOCK_W ** -0.5),
        'lru_br': nrm((L, 2, LRU_WIDTH), 0.01),
        'lru_wi': nrm((L, 2, LRU_BLOCKS, LRU_BLOCK_W, LRU_BLOCK_W), LRU_BLOCK_W ** -0.5),
        'lru_bi': nrm((L, 2, LRU_WIDTH), 0.01),
        'lru_lambda': jnp.log(root) - jnp.log1p(-root),
        'hgrn_lb_logits': nrm((L, 2, HG_K_W), 0.5),
        'hgrn_norm_g': gain((L, HG_V_W)),
        'w_out': nrm((L, MIX_W, D), MIX_W ** -0.5),
        'norm2_g': gain((L, D)),
        'router_w': nrm((L, D, N_EXPERTS), D ** -0.5),
        'router_b': nrm((L, N_EXPERTS), 0.01),
        'moe_w_gu': nrm((L, N_EXPERTS, D, 2 * D_FF), D ** -0.5),
        'moe_b_gu': nrm((L, N_EXPERTS, 2 * D_FF), 0.01),
        'moe_w_down': nrm((L, N_EXPERTS, D_FF, D), D_FF ** -0.5),
        'moe_b_down': nrm((L, N_EXPERTS, D), 0.01),
        'final_g': gain((D,)),
    }


def reference(x, c, ctx, c_ctx, ada_w, ada_b, norm1_g, w_in, attn_sink, conv_w, conv_b,
              lru_wr, lru_br, lru_wi, lru_bi, lru_lambda, hgrn_lb_logits, hgrn_norm_g, w_out,
              norm2_g, router_w, router_b, moe_w_gu, moe_b_gu, moe_w_down, moe_b_down, final_g):
    bsz, seq, d = x.shape
    ctx_len = ctx.shape[1]
    cos, sin = axial_rope_tables(seq, x.dtype)
    lb_p = jax.nn.softmax(hgrn_lb_logits.astype(jnp.float32), axis=0)
    lower_bounds = jnp.cumsum(lb_p, axis=0) - lb_p[0]
    cond_l = jax.nn.silu(c)
    cond_c = jax.nn.silu(c_ctx)
    cuts = np.cumsum(IN_SPLITS)[:-1].tolist()
    xl, xc = x, ctx
    for layer in range(DEPTH):
        ctx_out = layer < DEPTH - 1
        mod_l = jnp.split((cond_l @ ada_w[layer] + ada_b[layer])[:, None, :], 6, axis=-1)
        mod_c = jnp.split(cond_c @ ada_w[layer] + ada_b[layer], 6, axis=-1)
        hl = modulate(rms_norm(xl, norm1_g[layer]), mod_l[0], mod_l[1])
        hc = modulate(rms_norm(xc, norm1_g[layer]), mod_c[0], mod_c[1])
        pl = jnp.split(hl @ w_in[layer], cuts, axis=-1)
        pc = jnp.split(hc @ w_in[layer], cuts, axis=-1)
        att_l, att_c = window_attention(pl[0], pl[1], pl[2], pc[0], pc[1], pc[2],
                                        attn_sink[layer], cos, sin, ctx_out)
        lru_l, lru_c = rglru_mixer(pl[3], pl[4], pc[3], pc[4], conv_w[layer], conv_b[layer],
                                   lru_wr[layer], lru_br[layer], lru_wi[layer], lru_bi[layer],
                                   lru_lambda[layer], ctx_out)
        hg_l, hg_c = hgrn2_mixer(pl[5:], pc[5:], lower_bounds[layer], hgrn_norm_g[layer], ctx_out)
        xl = xl + mod_l[2] * (jnp.concatenate([att_l, lru_l, hg_l], axis=-1) @ w_out[layer])
        hl = modulate(rms_norm(xl, norm2_g[layer]), mod_l[3], mod_l[4])
        tokens = hl.reshape(bsz * seq, d)
        if ctx_out:
            xc = xc + mod_c[2] * (jnp.concatenate([att_c, lru_c, hg_c], axis=-1) @ w_out[layer])
            hc = modulate(rms_norm(xc, norm2_g[layer]), mod_c[3], mod_c[4])
            tokens = jnp.concatenate([tokens, hc.reshape(bsz * ctx_len, d)], axis=0)
        ffn = moe_ffn(tokens, router_w[layer], router_b[layer], moe_w_gu[layer], moe_b_gu[layer],
                      moe_w_down[layer], moe_b_down[layer])
        xl = xl + mod_l[5] * ffn[:bsz * seq].reshape(bsz, seq, d)
        if ctx_out:
            xc = xc + mod_c[5] * ffn[bsz * seq:].reshape(bsz, ctx_len, d)
    return rms_norm(xl, final_g)
```

```python
import numpy as np
import concourse.bass as bass
import concourse.mybir as mybir

F32 = mybir.dt.float32
BF16 = mybir.dt.bfloat16
AF = mybir.ActivationFunctionType
ALU = mybir.AluOpType
AX = mybir.AxisListType


class Trk:
    __slots__ = ("w", "r", "name")

    def __init__(self, name=""):
        self.w = None
        self.r = []
        self.name = name


class V:
    __slots__ = ("ap", "trk")

    def __init__(self, ap, trk):
        self.ap = ap
        self.trk = trk


class TT:
    def __init__(self, handle, name="", trk=None):
        self.h = handle
        self.trk = trk or Trk(name)
        self.subs = {}

    def __getitem__(self, idx):
        return V(self.h[idx], self.trk)

    def sub(self, key, idx):
        if key not in self.subs:
            self.subs[key] = Trk(f"{self.trk.name}.{key}")
        return V(self.h[idx], self.subs[key])


class K:
    def __init__(self, nc):
        self.nc = nc
        self.es = {}
        for nm, e in (("pe", nc.tensor), ("dve", nc.vector), ("act", nc.scalar), ("pool", nc.gpsimd), ("sp", nc.sync)):
            self.es[nm] = dict(e=e, sem=None, cnt=0, seen={}, name=nm)
        self._cm = []
        self.dma_sems = {}
        self.n_inst = 0

    def enter(self, cm):
        v = cm.__enter__()
        self._cm.append(cm)
        return v

    def close(self):
        for cm in reversed(self._cm):
            cm.__exit__(None, None, None)
        self._cm = []

    def sem(self, name):
        return self.enter(self.nc.semaphore(name))

    def sbuf(self, name, shape, dt=F32):
        return TT(self.enter(self.nc.sbuf_tensor(name, list(shape), dt)), name)

    def psum(self, name, shape, dt=F32):
        return TT(self.enter(self.nc.psum_tensor(name, list(shape), dt)), name)

    def dram(self, name, shape, dt=F32, kind="ExternalInput"):
        return TT(self.nc.dram_tensor(name, list(shape), dt, kind=kind).ap(), name)

    def _esem(self, E):
        if E["sem"] is None:
            E["sem"] = self.sem("p_" + E["name"])
        return E["sem"]

    def _wait(self, E, ev):
        sem, val, src = ev
        key = id(sem)
        if E["seen"].get(key, 0) >= val:
            return
        E["seen"][key] = val
        E["e"].wait_ge(sem, val)

    def _deps(self, E, ins, outs, skip_pe_waw=False):
        for v in ins:
            t = v.trk
            if t.w is not None:
                self._wait(E, t.w)
        for v in outs:
            t = v.trk
            if t.w is not None:
                if not (skip_pe_waw and t.w[2] == "pe" and E["name"] == "pe"):
                    self._wait(E, t.w)
            for ev in t.r:
                if ev[2] == E["name"] and E["name"] == "pe":
                    continue
                self._wait(E, ev)

    def _commit(self, ev, ins, outs):
        for v in ins:
            v.trk.r.append(ev)
            if len(v.trk.r) > 64:
                best = {}
                for e2 in v.trk.r:
                    k = id(e2[0])
                    if k not in best or best[k][1] < e2[1]:
                        best[k] = e2
                v.trk.r = list(best.values())
        for v in outs:
            v.trk.w = ev
            v.trk.r = []

    def op(self, en, fn, ins, outs, skip_pe_waw=False):
        E = self.es[en]
        sem = self._esem(E)
        self._deps(E, ins, outs, skip_pe_waw)
        inst = fn(E["e"])
        E["cnt"] += 1
        inst.then_inc(sem, 1)
        self.n_inst += 1
        ev = (sem, E["cnt"], en)
        self._commit(ev, ins, outs)
        return inst

    def dma(self, out, in_, en="sp", **kw):
        E = self.es[en]
        self._deps(E, [in_], [out])
        key = id(out.trk)
        if key not in self.dma_sems:
            self.dma_sems[key] = [self.sem("d%d" % len(self.dma_sems)), 0, out.trk]
        ent = self.dma_sems[key]
        ent[1] += 16
        E["e"].dma_start(out=out.ap, in_=in_.ap, **kw).then_inc(ent[0], 16)
        self.n_inst += 1
        ev = (ent[0], ent[1], "dma")
        self._commit(ev, [in_], [out])

    def wait_all_dma(self, en="sp"):
        E = self.es[en]
        for sem, val, trk in self.dma_sems.values():
            self._wait(E, (sem, val, "dma"))

    def mm(self, out, lhsT, rhs, start=True, stop=True, **kw):
        return self.op("pe", lambda e: e.matmul(out.ap, lhsT=lhsT.ap, rhs=rhs.ap, start=start, stop=stop, **kw),
                       [lhsT, rhs], [out], skip_pe_waw=not start)

    def transpose(self, out, in_, ident):
        return self.op("pe", lambda e: e.transpose(out.ap, in_.ap, ident.ap), [in_, ident], [out])

    def act(self, out, in_, func, bias=None, scale=None, en="act", accum_out=None):
        ins = [in_]
        kw = {}
        if bias is not None:
            if isinstance(bias, V):
                ins.append(bias); kw["bias"] = bias.ap
            else:
                kw["bias"] = float(bias)
        if scale is not None:
            if isinstance(scale, V):
                ins.append(scale); kw["scale"] = scale.ap
            else:
                kw["scale"] = float(scale)
        outs = [out]
        if accum_out is not None:
            outs.append(accum_out); kw["accum_out"] = accum_out.ap
        return self.op(en, lambda e: e.activation(out=out.ap, in_=in_.ap, func=func, **kw), ins, outs)

    def tt(self, out, a, b, op, en="dve"):
        return self.op(en, lambda e: e.tensor_tensor(out.ap, a.ap, b.ap, op=op), [a, b], [out])

    def ts(self, out, a, s1, s2, op0, op1=None, en="dve", accum_out=None):
        ins = [a]
        def cv(s):
            if isinstance(s, V):
                ins.append(s); return s.ap
            return s
        s1v = cv(s1); s2v = cv(s2)
        kw = {}
        if op1 is not None:
            kw["op1"] = op1
        outs = [out]
        if accum_out is not None:
            outs.append(accum_out); kw["accum_out"] = accum_out.ap
        return self.op(en, lambda e: e.tensor_scalar(out.ap, a.ap, s1v, s2v, op0=op0, **kw), ins, outs)

    def stt(self, out, a, s, b, op0, op1):
        ins = [a, b]
        sv = s
        if isinstance(s, V):
            ins.append(s); sv = s.ap
        return self.op("dve", lambda e: e.scalar_tensor_tensor(out.ap, a.ap, sv, b.ap, op0=op0, op1=op1), ins, [out])

    def copy(self, out, in_, en="dve"):
        if en == "act":
            return self.act(out, in_, AF.Copy)
        return self.op(en, lambda e: e.tensor_copy(out.ap, in_.ap), [in_], [out])

    def memset(self, out, val, en="dve"):
        return self.op(en, lambda e: e.memset(out.ap, val), [], [out])

    def scan(self, out, d0, d1, init, op0=ALU.mult, op1=ALU.add):
        ins = [d0, d1]
        iv = init
        if isinstance(init, V):
            ins.append(init); iv = init.ap
        return self.op("dve", lambda e: e.tensor_tensor_scan(out.ap, d0.ap, d1.ap, iv, op0=op0, op1=op1), ins, [out])

    def recip(self, out, in_):
        return self.op("dve", lambda e: e.reciprocal(out.ap, in_.ap), [in_], [out])

    def reduce(self, out, in_, op=ALU.add, axis=AX.X):
        return self.op("dve", lambda e: e.tensor_reduce(out.ap, in_.ap, axis=axis, op=op), [in_], [out])

    def max8(self, out, in_):
        return self.op("dve", lambda e: e.max(out=out.ap, in_=in_.ap), [in_], [out])

    def finish(self):
        E = self.es["sp"]
        self.wait_all_dma("sp")
        for nm in ("pe", "dve", "act", "pool"):
            X = self.es[nm]
            if X["sem"] is not None and X["cnt"] > 0:
                self._wait(E, (X["sem"], X["cnt"], nm))


def _k_mark(self):
    return len(self._cm)


def _k_release(self, mark):
    self.barrier()
    while len(self._cm) > mark:
        self._cm.pop().__exit__(None, None, None)


def _k_barrier(self):
    evs = []
    for nm in ("pe", "dve", "act", "pool"):
        X = self.es[nm]
        if X["sem"] is not None and X["cnt"] > 0:
            evs.append((X["sem"], X["cnt"], nm))
    for sem, val, trk in self.dma_sems.values():
        evs.append((sem, val, "dma"))
    for nm in ("pe", "dve", "act", "pool", "sp"):
        E = self.es[nm]
        for ev in evs:
            self._wait(E, ev)


K.mark = _k_mark
K.release = _k_release
K.barrier = _k_barrier


import numpy as np
from concourse.bass_utils import run_bass_kernel_spmd

D = 1024
NCORE = 8
EPS = 1e-6


def run(nc, in_maps):
    res = run_bass_kernel_spmd(nc, in_maps, core_ids=list(range(NCORE)))
    return res.results


def fm(vec):
    return np.ascontiguousarray(vec.reshape(8, 128).T)


def build_adaln():
    nc = bass.Bass("TRN2", target_bir_lowering=False)
    k = K(nc)
    condT = k.dram("condT", [128, 8, 3])
    w = k.dram("w", [2, 1024, 768])
    brep = k.dram("brep", [3, 2, 768])
    out = k.dram("mod", [3, 2, 768], kind="ExternalOutput")
    ct = k.sbuf("ct", [128, 8, 3])
    sg = k.sbuf("sg", [128, 8, 3])
    wt = k.sbuf("wt", [128, 2, 8, 768])
    bt = k.sbuf("bt", [3, 2, 768])
    ot = k.sbuf("ot", [3, 2, 768])
    k.dma(ct[:], condT[:, :, :])
    k.dma(bt[:], brep[:, :, :])
    for l in range(2):
        k.dma(wt[:, l, :, :], V(w.h[l].rearrange("(k p) n -> p k n", p=128), w.trk))
    k.act(sg[:], ct[:], AF.Sigmoid)
    k.tt(sg[:], sg[:], ct[:], ALU.mult)
    ps = [k.psum("ps%d" % i, [3, 384]) for i in range(2)]
    i = 0
    for l in range(2):
        for h in range(2):
            p = ps[i % 2]; i += 1
            for kk in range(8):
                k.mm(p[:], sg[:, kk, :], wt[:, l, kk, h * 384:(h + 1) * 384], start=kk == 0, stop=kk == 7)
            k.tt(ot[:, l, h * 384:(h + 1) * 384], p[:], bt[:, l, h * 384:(h + 1) * 384], ALU.add)
    k.dma(out[:, :, :], ot[:])
    k.finish(); k.close()
    return nc


def host_adaln(c, c_ctx, ada_w, ada_b):
    cond = np.concatenate([c, c_ctx[None]], 0)
    condT = np.ascontiguousarray(cond.T.reshape(8, 128, 3).transpose(1, 0, 2))
    nc = build_adaln()
    maps = []
    for ci in range(NCORE):
        sl = slice(ci * 768, (ci + 1) * 768)
        maps.append({"condT": condT, "w": np.ascontiguousarray(ada_w[:, :, sl]),
                     "brep": np.ascontiguousarray(np.broadcast_to(ada_b[None, :, sl], (3, 2, 768)))})
    r = run(nc, maps)
    mod = np.concatenate([r[ci]["mod"] for ci in range(NCORE)], axis=2)
    return mod


def modv_for_core(mod, layer, b):
    o = np.zeros((128, 8, 6, 2), np.float32)
    for gi, g in enumerate((b, 2)):
        v = mod[g, layer].reshape(6, 8, 128)
        o[:, :, :, gi] = v.transpose(2, 1, 0)
    return o


TILES = [(i * 512, 512, 0) for i in range(8)] + [(4096, 64, 1)]
TCORE = 4160


def setup_modscale(k, modv, g, vec_scale, vec_shift, name):
    gs = k.sbuf(name + "_gs", [128, 8, 2])
    for grp in range(2):
        k.ts(gs[:, :, grp], modv[:, :, vec_scale, grp], 1.0, None, ALU.add)
        k.tt(gs[:, :, grp], gs[:, :, grp], g[:], ALU.mult)
    return gs


def emit_norm(k, xt, n, grp, gs, modv, vec_shift, ones, ps_ss, wk, h_bf, h_f32=None):
    sq = wk["sq"]; rstd = wk["rstd"]; tmp = wk["tmp"]
    k.act(sq[:, :, 0:n], xt[:, :, 0:n], AF.Square)
    for kk in range(8):
        k.mm(ps_ss[:, 0:n], ones[:], sq[:, kk, 0:n], start=kk == 0, stop=kk == 7)
    k.act(rstd[:, 0:n], ps_ss[:, 0:n], AF.Sqrt, bias=wk["eps"][:], scale=1.0 / 1024)
    k.recip(rstd[:, 0:n], rstd[:, 0:n])
    for kk in range(8):
        t = tmp[kk % 2]
        k.tt(t[:, 0:n], xt[:, kk, 0:n], rstd[:, 0:n], ALU.mult)
        if h_f32 is not None:
            k.act(h_f32[:, kk, 0:n], t[:, 0:n], AF.Identity, bias=modv[:, kk, vec_shift:vec_shift + 1, grp][:, :, 0] if False else V(modv.h[:, kk, vec_shift, grp:grp + 1], modv.trk),
                  scale=V(gs.h[:, kk, grp:grp + 1], gs.trk))
            k.copy(h_bf[:, kk, 0:n], h_f32[:, kk, 0:n], en="pool")
        else:
            k.act(h_bf[:, kk, 0:n], t[:, 0:n], AF.Identity, bias=V(modv.h[:, kk, vec_shift, grp:grp + 1], modv.trk),
                  scale=V(gs.h[:, kk, grp:grp + 1], gs.trk))


def norm_work(k, pre):
    wk = {"sq": k.sbuf(pre + "sq", [128, 8, 512]), "rstd": k.sbuf(pre + "rstd", [128, 512]),
          "tmp": [k.sbuf(pre + "tmp%d" % i, [128, 512]) for i in range(2)],
          "eps": k.sbuf(pre + "eps", [128, 1])}
    k.memset(wk["eps"][:], EPS)
    return wk


def build_inproj():
    nc = bass.Bass("TRN2", target_bir_lowering=False)
    k = K(nc)
    xT = k.dram("xT", [1024, TCORE])
    w = k.dram("w_in", [1024, 2560])
    g1 = k.dram("g1", [128, 8])
    modv_d = k.dram("modv", [128, 8, 6, 2])
    pT = k.dram("pT", [2560, TCORE], kind="ExternalOutput")
    modv = k.sbuf("modv_s", [128, 8, 6, 2]); k.dma(modv[:], modv_d[:, :, :, :])
    g1s = k.sbuf("g1s", [128, 8]); k.dma(g1s[:], g1[:, :])
    gs = setup_modscale(k, modv, g1s, 1, 0, "n1")
    ones = k.sbuf("ones", [128, 128]); k.memset(ones[:], 1.0)
    wk = norm_work(k, "n1")
    w_bf = k.sbuf("w_bf", [128, 8, 2560], BF16)
    stg = [k.sbuf("stg%d" % i, [128, 2560]) for i in range(2)]
    for kk in range(8):
        s = stg[kk % 2]
        k.dma(s[:], w[kk * 128:(kk + 1) * 128, :])
        k.copy(w_bf[:, kk, :], s[:], en="pool")
    xts = [k.sbuf("xt%d" % i, [128, 8, 512]) for i in range(2)]
    hbs = [k.sbuf("hb%d" % i, [128, 8, 512], BF16) for i in range(2)]
    ot = k.sbuf("ot", [128, 20, 512])
    ps_ss = k.psum("ps_ss", [128, 512])
    pss = [k.psum("ps%d" % i, [128, 512]) for i in range(4)]
    xv = lambda c0, n: V(xT.h[:, c0:c0 + n].rearrange("(k p) n -> p k n", p=128), xT.trk)
    for ti, (c0, n, grp) in enumerate(TILES):
        xt = xts[ti % 2]; hb = hbs[ti % 2]
        k.dma(xt[:, :, 0:n], xv(c0, n))
        emit_norm(k, xt, n, grp, gs, modv, 0, ones, ps_ss, wk, hb)
        for m in range(20):
            p = pss[m % 4]
            for kk in range(8):
                k.mm(p[:, 0:n], w_bf[:, kk, m * 128:(m + 1) * 128], hb[:, kk, 0:n], start=kk == 0, stop=kk == 7)
            k.copy(ot[:, m, 0:n], p[:, 0:n], en="act" if m % 2 else "dve")
        k.dma(V(pT.h[:, c0:c0 + n].rearrange("(m p) n -> p m n", p=128), pT.trk), ot[:, :, 0:n])
    k.finish(); k.close()
    return nc


def core_tokens_T(xl, xc, ci):
    b, q = ci // 4, ci % 4
    return np.ascontiguousarray(np.concatenate([xl[b, q * 4096:(q + 1) * 4096], xc[b, q * 64:(q + 1) * 64]], 0).T)


def scatter_tokens(outs, W):
    l = np.zeros((2, 16384, W), np.float32); c = np.zeros((2, 256, W), np.float32)
    for ci, o in enumerate(outs):
        b, q = ci // 4, ci % 4
        l[b, q * 4096:(q + 1) * 4096] = o[:, :4096].T
        c[b, q * 64:(q + 1) * 64] = o[:, 4096:].T
    return l, c


def host_inproj(nc, xl, xc, w_in_l, g1_l, mod, layer):
    maps = []
    for ci in range(NCORE):
        maps.append({"xT": core_tokens_T(xl, xc, ci), "w_in": w_in_l, "g1": fm(g1_l),
                     "modv": modv_for_core(mod, layer, ci // 4)})
    r = run(nc, maps)
    return scatter_tokens([r[ci]["pT"] for ci in range(NCORE)], 2560)


import numpy as np

TF = 16640
TL = 16384
NBLK = 128


def ident_tile(k, name, n=128, dt=F32):
    t = k.sbuf(name, [n, n], dt)
    k.memset(t[:], 0.0)
    k.op("pool", lambda g: g.affine_select(out=t.h[:], in_=t.h[:], pattern=[[-1, n]], compare_op=ALU.not_equal,
                                           fill=1.0, base=0, channel_multiplier=1), [t[:]], [t[:]])
    return t


def rope_tables():
    t = np.arange(TL)
    rows = (t // 64).astype(np.float32); cols = (t % 64).astype(np.float32)
    inv = (10000.0 ** (-np.arange(16, dtype=np.float32) / 16)).astype(np.float32)
    C = np.zeros((64, TL), np.float32); S = np.zeros((64, TL), np.float32)
    for a, pos in enumerate((rows, cols)):
        ang = (pos[None, :] * inv[:, None]).astype(np.float32)
        cs, sn = np.cos(ang).astype(np.float32), np.sin(ang).astype(np.float32)
        C[a * 32:a * 32 + 16] = cs; C[a * 32 + 16:a * 32 + 32] = cs
        S[a * 32:a * 32 + 16] = -sn; S[a * 32 + 16:a * 32 + 32] = sn
    return C, S


SWAP = np.concatenate([np.arange(16, 32), np.arange(0, 16), np.arange(48, 64), np.arange(32, 48)])


def build_attn(ctx_out):
    nc = bass.Bass("TRN2", target_bir_lowering=False)
    k = K(nc)
    qT = k.dram("qT", [64, 2, TF]); qsw = k.dram("qsw", [64, 2, TL])
    kT = k.dram("kT", [64, TF]); ksw = k.dram("ksw", [64, TL])
    vd = k.dram("v", [TF, 64])
    Cd = k.dram("ropeC", [64, TL]); Sd = k.dram("ropeS", [64, TL])
    sinkd = k.dram("sink", [64, 2])
    out = k.dram("attT", [64, 2, TF], kind="ExternalOutput")
    q_bf = k.sbuf("q_bf", [64, 2, TF], BF16)
    k_bf = k.sbuf("k_bf", [64, TF], BF16)
    v_bf = k.sbuf("v_bf", [128, 130, 64], BF16)
    ones_bf = k.sbuf("ones_bf", [128, 64], BF16); k.memset(ones_bf[:], 1.0)
    es = k.sbuf("es", [64, 2]); k.dma(es[:], sinkd[:, :]); k.act(es[:], es[:], AF.Exp)
    mprev = k.sbuf("mprev", [128, 2, 128], BF16); mnext = k.sbuf("mnext", [128, 2, 128], BF16)
    for m, cm, st in ((mprev, 1, -1), (mnext, -1, 1)):
        k.memset(m[:], 1.0)
        k.op("pool", lambda g, m=m, cm=cm, st=st: g.affine_select(out=m.h[:], in_=m.h[:], pattern=[[0, 2], [st, 128]],
                                                                  compare_op=ALU.is_ge, fill=0.0, base=0, channel_multiplier=cm),
             [m[:]], [m[:]])
    vst = [k.sbuf("vst%d" % i, [128, 26, 64]) for i in range(2)]
    for i in range(5):
        s = vst[i % 2]
        k.dma(s[:], V(vd.h[i * 3328:(i + 1) * 3328, :].rearrange("(t p) d -> p t d", p=128), vd.trk))
        k.copy(v_bf[:, i * 26:(i + 1) * 26, :], s[:], en="pool")
    cq = k.sbuf("cq", [64, 2, 256]); ck = k.sbuf("ck", [64, 256])
    k.dma(cq[:], qT[:, :, 0:256]); k.dma(ck[:], kT[:, 0:256])
    k.ts(q_bf[:, :, 0:256], cq[:], 0.125, None, ALU.mult)
    k.copy(k_bf[:, 0:256], ck[:])
    NB = 2
    qa = [k.sbuf("qa%d" % i, [64, 2, 512]) for i in range(NB)]; qb = [k.sbuf("qb%d" % i, [64, 2, 512]) for i in range(NB)]
    ka = [k.sbuf("ka%d" % i, [64, 512]) for i in range(NB)]; kb_ = [k.sbuf("kb%d" % i, [64, 512]) for i in range(NB)]
    Ct = [k.sbuf("Ct%d" % i, [64, 2, 512]) for i in range(NB)]; St = [k.sbuf("St%d" % i, [64, 2, 512]) for i in range(NB)]
    for ti in range(32):
        c0 = ti * 512; f0 = 256 + c0; i = ti % NB
        k.dma(qa[i][:], qT[:, :, f0:f0 + 512]); k.dma(qb[i][:], qsw[:, :, c0:c0 + 512])
        k.dma(ka[i][:], kT[:, f0:f0 + 512]); k.dma(kb_[i][:], ksw[:, c0:c0 + 512])
        for h in range(2):
            k.dma(Ct[i][:, h, :], Cd[:, c0:c0 + 512]); k.dma(St[i][:, h, :], Sd[:, c0:c0 + 512])
        k.tt(qa[i][:], qa[i][:], Ct[i][:], ALU.mult)
        k.tt(qb[i][:], qb[i][:], St[i][:], ALU.mult, en="pool")
        k.tt(qa[i][:], qa[i][:], qb[i][:], ALU.add)
        k.act(q_bf[:, :, f0:f0 + 512], qa[i][:], AF.Copy, scale=0.125)
        k.tt(ka[i][:], ka[i][:], Ct[i][:, 0, :], ALU.mult)
        k.tt(kb_[i][:], kb_[i][:], St[i][:, 0, :], ALU.mult, en="pool")
        k.tt(k_bf[:, f0:f0 + 512], ka[i][:], kb_[i][:], ALU.add)
    sc = [k.psum("sc%d" % i, [128, 6, 256]) for i in range(2)]
    po = k.psum("po", [64, 512]); pd = k.psum("pd", [64, 512])
    Es = [k.sbuf("E%d" % i, [128, 5, 256], BF16) for i in range(2)]
    den = k.sbuf("den", [64, 256])
    ob = [k.sbuf("ob%d" % i, [64, 2, 512]) for i in range(2)]
    blocks = []
    if ctx_out:
        blocks += [("c", 0), ("c", 1)]
    blocks += [("l", i) for i in range(NBLK)]
    if not ctx_out:
        zt = k.sbuf("zt", [64, 2, 256]); k.memset(zt[:], 0.0); k.dma(out[:, :, 0:256], zt[:])
    for bi, (kind, i) in enumerate(blocks):
        if kind == "c":
            qtile = i; tiles = [(0, None), (1, None)]
        else:
            qtile = 2 + i
            tiles = [(0, None), (1, None), (2 + i, None)]
            if i > 0:
                tiles.append((2 + i - 1, mprev))
            if i < NBLK - 1:
                tiles.append((2 + i + 1, mnext))
        nt = len(tiles)
        s = sc[bi % 2]; E = Es[bi % 2]
        qv = q_bf[:, :, qtile * 128:(qtile + 1) * 128]
        for j, (kt, _) in enumerate(tiles):
            k.mm(s[:, j, :], k_bf[:, kt * 128:(kt + 1) * 128], qv)
        k.act(E[:, 0:nt, :], s[:, 0:nt, :], AF.Exp)
        for j, (kt, m) in enumerate(tiles):
            if m is not None:
                k.tt(E[:, j, :], E[:, j, :], V(m.h[:].rearrange("p a b -> p (a b)"), m.trk), ALU.mult, en="pool")
        for j, (kt, _) in enumerate(tiles):
            k.mm(po[:, 0:256], v_bf[:, kt, :], E[:, j, :], start=j == 0, stop=j == nt - 1)
        for j, (kt, _) in enumerate(tiles):
            k.mm(pd[:, 0:256], ones_bf[:], E[:, j, :], start=j == 0, stop=j == nt - 1)
        for h in range(2):
            k.ts(den[:, h * 128:(h + 1) * 128], pd[:, h * 128:(h + 1) * 128], es[:, h:h + 1], None, ALU.add)
        k.recip(den[:], den[:])
        if kind == "c":
            o = ob[0]; sub = i; ncols = 256; base = 0; last = (i == 1)
        else:
            o = ob[(i // 4) % 2]; sub = i % 4; ncols = 512; base = 256 + (i // 4) * 512; last = (sub == 3)
        k.tt(o[:, :, sub * 128:(sub + 1) * 128], V(po.h[:, 0:256].rearrange("p (h q) -> p h q", h=2), po.trk),
             V(den.h[:].rearrange("p (h q) -> p h q", h=2), den.trk), ALU.mult)
        if last:
            k.dma(out[:, :, base:base + ncols], o[:, :, 0:ncols])
    k.finish(); k.close()
    return nc


def attn_maps(pl, pc, sink_l):
    C, S = rope_tables()
    maps = []
    for ci in range(NCORE):
        b, j = ci // 4, ci % 4
        full = np.concatenate([pc[b], pl[b]], 0)
        q = full[:, 0:512].reshape(TF, 8, 64)[:, 2 * j:2 * j + 2]
        kv = j // 2
        kk = full[:, 512 + kv * 64:512 + kv * 64 + 64]
        v = full[:, 640 + kv * 64:640 + kv * 64 + 64]
        qT = np.ascontiguousarray(q.transpose(2, 1, 0))
        kT = np.ascontiguousarray(kk.T)
        maps.append({"qT": qT, "qsw": np.ascontiguousarray(qT[SWAP][:, :, 256:]), "kT": kT,
                     "ksw": np.ascontiguousarray(kT[SWAP][:, 256:]), "v": np.ascontiguousarray(v),
                     "ropeC": C, "ropeS": S,
                     "sink": np.ascontiguousarray(np.broadcast_to(sink_l[2 * j:2 * j + 2][None], (64, 2)))})
    return maps


import numpy as np

LT = 1024


def build_lru():
    nc = bass.Bass("TRN2", target_bir_lowering=False)
    k = K(nc)
    xpl = k.dram("xpl", [64, TL + 4]); xpc = k.dram("xpc", [64, 256 + 4])
    gT = k.dram("gT", [64, TF])
    cw = k.dram("cw", [64, 4]); cb = k.dram("cb", [64, 1])
    wr = k.dram("wr", [64, 2, 64]); wi = k.dram("wi", [64, 2, 64])
    br = k.dram("br", [64, 2]); bi = k.dram("bi", [64, 2]); lam = k.dram("lam", [64, 2])
    out = k.dram("lruT", [64, TF], kind="ExternalOutput")
    sb = lambda n, s: k.sbuf(n, s)
    cws = sb("cws", [64, 4]); cbs = sb("cbs", [64, 1]); wrs = sb("wrs", [64, 2, 64]); wis = sb("wis", [64, 2, 64])
    brs = sb("brs", [64, 2]); bis = sb("bis", [64, 2]); lams = sb("lams", [64, 2]); c8 = sb("c8", [64, 2])
    for s, d in ((cws, cw), (cbs, cb), (brs, br), (bis, bi), (lams, lam)):
        k.dma(s[:], d[:, :])
    k.dma(wrs[:], wr[:, :, :]); k.dma(wis[:], wi[:, :, :])
    k.act(c8[:], lams[:], AF.Exp, scale=-1.0)
    k.act(c8[:], c8[:], AF.Ln, bias=1.0)
    k.ts(c8[:], c8[:], -8.0, None, ALU.mult)
    Hf = sb("Hf", [64, TF])
    NB = 2
    xp = [sb("xp%d" % i, [64, LT + 4]) for i in range(NB)]
    u = [sb("u%d" % i, [64, LT]) for i in range(NB)]
    r = [sb("r%d" % i, [64, LT]) for i in range(NB)]
    ii = [sb("i%d" % i, [64, LT]) for i in range(NB)]
    a = [sb("a%d" % i, [64, LT]) for i in range(NB)]
    m2 = [sb("m2%d" % i, [64, LT]) for i in range(NB)]
    bb = [sb("bb%d" % i, [64, LT]) for i in range(NB)]
    hb = [sb("hb%d" % i, [64, LT]) for i in range(NB)]
    g = [sb("g%d" % i, [64, LT]) for i in range(NB)]
    g2 = [sb("g2%d" % i, [64, LT]) for i in range(NB)]
    y = [sb("y%d" % i, [64, LT]) for i in range(NB)]
    psr = [k.psum("psr%d" % i, [64, LT]) for i in range(2)]
    psi = [k.psum("psi%d" % i, [64, LT]) for i in range(2)]
    tiles = [(0, 256, xpc, 0)] + [(256 + t * LT, LT, xpl, t * LT) for t in range(TL // LT)]
    cnt = 0
    for d in range(2):
        order = tiles if d == 0 else [tiles[0]] + tiles[:0:-1]
        prev_hb = None
        for (f0, n, src, s0) in order:
            i = cnt % NB; cnt += 1
            k.dma(xp[i][:, 0:n + 4], src[:, s0:s0 + n + 4])
            k.ts(u[i][:, 0:n], xp[i][:, 0:n], cws[:, 0:1], cbs[:, 0:1], ALU.mult, ALU.add)
            for tap in range(1, 4):
                k.stt(u[i][:, 0:n], xp[i][:, tap:tap + n], cws[:, tap:tap + 1], u[i][:, 0:n], ALU.mult, ALU.add)
            for c in range(0, n, 512):
                w_ = min(512, n - c)
                k.mm(psr[i][:, c:c + w_], wrs[:, d, :], u[i][:, c:c + w_])
                k.mm(psi[i][:, c:c + w_], wis[:, d, :], u[i][:, c:c + w_])
            k.act(r[i][:, 0:n], psr[i][:, 0:n], AF.Sigmoid, bias=brs[:, d:d + 1])
            k.act(ii[i][:, 0:n], psi[i][:, 0:n], AF.Sigmoid, bias=bis[:, d:d + 1])
            k.act(a[i][:, 0:n], r[i][:, 0:n], AF.Exp, scale=c8[:, d:d + 1])
            k.stt(m2[i][:, 0:n], a[i][:, 0:n], -1.0, a[i][:, 0:n], ALU.mult, ALU.mult)
            k.ts(m2[i][:, 0:n], m2[i][:, 0:n], 1.0, 1e-12, ALU.add, ALU.max)
            k.act(m2[i][:, 0:n], m2[i][:, 0:n], AF.Sqrt)
            k.tt(bb[i][:, 0:n], ii[i][:, 0:n], u[i][:, 0:n], ALU.mult, en="pool")
            k.tt(bb[i][:, 0:n], bb[i][:, 0:n], m2[i][:, 0:n], ALU.mult, en="pool")
            if d == 0:
                init = 0.0 if f0 == 0 else Hf[:, f0 - 1:f0]
                k.scan(Hf[:, f0:f0 + n], a[i][:, 0:n], bb[i][:, 0:n], init)
            else:
                init = 0.0 if prev_hb is None else prev_hb
                k.scan(hb[i][:, n - 1::-1] if False else V(hb[i].h[:, 0:n][:, ::-1], hb[i].trk),
                       V(a[i].h[:, 0:n][:, ::-1], a[i].trk), V(bb[i].h[:, 0:n][:, ::-1], bb[i].trk), init)
                prev_hb = hb[i][:, 0:1]
                k.dma(g[i][:, 0:n], gT[:, f0:f0 + n])
                k.tt(g2[i][:, 0:n], g[i][:, 0:n], g[i][:, 0:n], ALU.mult, en="pool")
                k.ts(g2[i][:, 0:n], g2[i][:, 0:n], 0.044715, 1.0, ALU.mult, ALU.add)
                k.tt(g2[i][:, 0:n], g2[i][:, 0:n], g[i][:, 0:n], ALU.mult, en="pool")
                k.act(g2[i][:, 0:n], g2[i][:, 0:n], AF.Sigmoid, scale=1.5957691216057308)
                k.tt(g2[i][:, 0:n], g2[i][:, 0:n], g[i][:, 0:n], ALU.mult, en="pool")
                k.tt(y[i][:, 0:n], Hf[:, f0:f0 + n], hb[i][:, 0:n], ALU.add)
                k.tt(y[i][:, 0:n], y[i][:, 0:n], g2[i][:, 0:n], ALU.mult)
                k.dma(out[:, f0:f0 + n], y[i][:, 0:n])
    k.finish(); k.close()
    return nc


def lru_maps(pl, pc, z, layer):
    maps = []
    for ci in range(NCORE):
        b, j = ci // 4, ci % 4
        sl = slice(768 + 64 * j, 768 + 64 * j + 64); gl = slice(1024 + 64 * j, 1024 + 64 * j + 64)
        xl = np.zeros((64, TL + 4), np.float32); xl[:, 2:2 + TL] = pl[b][:, sl].T
        xc = np.zeros((64, 260), np.float32); xc[:, 2:258] = pc[b][:, sl].T
        gT = np.ascontiguousarray(np.concatenate([pc[b][:, gl], pl[b][:, gl]], 0).T)
        cs = slice(64 * j, 64 * j + 64)
        maps.append({"xpl": xl, "xpc": xc, "gT": gT,
                     "cw": np.ascontiguousarray(z['conv_w'][layer][:, cs].T), "cb": np.ascontiguousarray(z['conv_b'][layer][cs][:, None]),
                     "wr": np.ascontiguousarray(z['lru_wr'][layer][:, j].transpose(1, 0, 2)),
                     "wi": np.ascontiguousarray(z['lru_wi'][layer][:, j].transpose(1, 0, 2)),
                     "br": np.ascontiguousarray(z['lru_br'][layer][:, cs].T), "bi": np.ascontiguousarray(z['lru_bi'][layer][:, cs].T),
                     "lam": np.ascontiguousarray(z['lru_lambda'][layer][:, cs].T)})
    return maps


import numpy as np

HT = 512


def build_hgrn(layer):
    nc = bass.Bass("TRN2", target_bir_lowering=False)
    k = K(nc)
    qT = k.dram("qT", [64, TF]); zT = [k.dram("zfT", [64, TF]), k.dram("zbT", [64, TF])]; ogT = k.dram("ogT", [64, TF])
    vd = k.dram("v", [TF, 64]); lbl = k.dram("lbl", [64, 2, 2]); ngd = k.dram("ng", [64, 1])
    out = k.dram("hgT", [64, TF], kind="ExternalOutput")
    sb = lambda n, s: k.sbuf(n, s)
    lbs = sb("lbs", [64, 2, 2]); k.dma(lbs[:], lbl[:, :, :])
    ng = sb("ng_s", [64, 1]); k.dma(ng[:], ngd[:, :])
    lb = sb("lb", [64, 2]); oml = sb("oml", [64, 2]); noml = sb("noml", [64, 2]); ssum = sb("ssum", [64, 2])
    k.act(lbs[:], lbs[:], AF.Exp)
    k.tt(ssum[:], lbs[:, 0, :], lbs[:, 1, :], ALU.add)
    k.recip(ssum[:], ssum[:])
    if layer == 0:
        k.memset(lb[:], 0.0)
    else:
        k.tt(lb[:], lbs[:, 1, :], ssum[:], ALU.mult)
    k.ts(oml[:], lb[:], -1.0, 1.0, ALU.mult, ALU.add)
    k.ts(noml[:], oml[:], -1.0, None, ALU.mult)
    eps = sb("eps_s", [64, 1]); k.memset(eps[:], 1e-6)
    ones64 = sb("ones64", [64, 64]); k.memset(ones64[:], 1.0)
    ident = ident_tile(k, "ident", 64)
    cms = []
    for d in range(2):
        cm = sb("cm%d" % d, [64, HT]); k.memset(cm[:], 1.0)
        pos = 0 if d == 0 else 31
        k.memset(V(cm.h[:].rearrange("p (c t) -> p c t", t=32)[:, :, pos:pos + 1], cm.trk), 0.0)
        cms.append(cm)
    masks = []
    for d in range(2):
        m = sb("mask%d" % d, [128, 128]); k.memset(m[:], 1.0)
        st, cmul = (1, -1) if d == 0 else (-1, 1)
        k.op("pool", lambda g, m=m, st=st, cmul=cmul: g.affine_select(out=m.h[:], in_=m.h[:], pattern=[[st, 128]], compare_op=ALU.is_ge,
                                                                     fill=0.0, base=0, channel_multiplier=cmul), [m[:]], [m[:]])
        for c in range(4):
            if d == 0:
                base, cmul2 = -32 * c, 1
            else:
                base, cmul2 = 32 * c + 31, -1
            k.op("pool", lambda g, m=m, c=c, base=base, cmul2=cmul2: g.affine_select(
                out=m.h[:, 32 * c:32 * c + 32], in_=m.h[:, 32 * c:32 * c + 32], pattern=[[0, 32]], compare_op=ALU.is_ge,
                fill=0.0, base=base, channel_multiplier=cmul2), [m[:]], [m[:]])
        masks.append(m)
    cm4 = sb("cm4", [128, 4]); k.memset(cm4[:], 1.0)
    for (st, cmul, base) in ((-32, 1, 0), (32, -1, 31)):
        k.op("pool", lambda g, st=st, cmul=cmul, base=base: g.affine_select(out=cm4.h[:], in_=cm4.h[:], pattern=[[st, 4]], compare_op=ALU.is_ge,
                                                                           fill=0.0, base=base, channel_multiplier=cmul), [cm4[:]], [cm4[:]])
    v_tm = sb("v_tm", [128, 130, 64])
    for i in range(5):
        k.dma(v_tm[:, i * 26:(i + 1) * 26, :], V(vd.h[i * 3328:(i + 1) * 3328, :].rearrange("(t p) d -> p t d", p=128), vd.trk))
    Of = sb("Of", [64, TF])
    R = 8
    Sr = sb("Sring", [64, R, 64])
    NB = 2
    mk = lambda nm: [sb("%s%d" % (nm, i), [64, HT]) for i in range(NB)]
    z_, q_, sg, lf, kk, b_, d1, Eq, Ek, Es, Ee, osum, ogt, yt = (mk(n) for n in
        ("z", "q", "sg", "lf", "kk", "b", "d1", "Eq", "Ek", "Es", "Ee", "osum", "ogt", "yt"))
    bls = [sb("bls%d" % i, [64, 16]) for i in range(NB)]; hbl = [sb("hbl%d" % i, [64, 16]) for i in range(NB)]
    dec = [sb("dec%d" % i, [64, 16]) for i in range(NB)]
    attm = [sb("attm%d" % i, [128, 128]) for i in range(2)]
    ke_tm = [sb("ketm%d" % i, [128, 4, 64]) for i in range(2)]
    rstd = sb("rstd", [64, HT])
    ps_att = [k.psum("psatt%d" % i, [128, 512]) for i in range(2)]
    ps_tr = k.psum("pstr", [128, 512])
    ps_kv = [k.psum("pskv%d" % i, [64, 512]) for i in range(2)]
    ps_o = [k.psum("pso%d" % i, [64, 512]) for i in range(2)]
    ps_ss = k.psum("psss", [64, 512])
    tiles = [(0, 256)] + [(256 + t * HT, HT) for t in range(TL // HT)]
    cnt = 0; scnt = 0
    for d in range(2):
        order = tiles if d == 0 else [tiles[0]] + tiles[:0:-1]
        slot = 0
        k.memset(Sr.sub(0, (slice(None), 0, slice(None))), 0.0)
        last = 31 if d == 0 else 0
        for (f0, n) in order:
            i = cnt % NB; cnt += 1
            nch = n // 32; nsub = n // 128
            k.dma(z_[i][:, 0:n], zT[d][:, f0:f0 + n]); k.dma(q_[i][:, 0:n], qT[:, f0:f0 + n])
            k.act(sg[i][:, 0:n], z_[i][:, 0:n], AF.Sigmoid)
            k.ts(lf[i][:, 0:n], sg[i][:, 0:n], oml[:, d:d + 1], lb[:, d:d + 1], ALU.mult, ALU.add)
            k.act(lf[i][:, 0:n], lf[i][:, 0:n], AF.Ln)
            k.ts(kk[i][:, 0:n], sg[i][:, 0:n], noml[:, d:d + 1], oml[:, d:d + 1], ALU.mult, ALU.add)
            if d == 0:
                k.scan(b_[i][:, 0:n], cms[0][:, 0:n], lf[i][:, 0:n], 0.0)
            else:
                rv = lambda t, n=n: V(t.h[:, 0:n][:, ::-1], t.trk)
                k.scan(rv(b_[i]), rv(cms[1]), rv(lf[i]), 0.0)
            b3 = V(b_[i].h[:, 0:n].rearrange("p (c t) -> p c t", t=32), b_[i].trk)
            v3 = lambda t, n=n: V(t.h[:, 0:n].rearrange("p (c t) -> p c t", t=32), t.trk)
            k.copy(V(bls[i].h[:, 0:nch].rearrange("p (c o) -> p c o", o=1), bls[i].trk),
                   V(b_[i].h[:, 0:n].rearrange("p (c t) -> p c t", t=32)[:, :, last:last + 1], b_[i].trk))
            k.ts(hbl[i][:, 0:nch], bls[i][:, 0:nch], 0.5, None, ALU.mult)
            bc = lambda t, nch=nch: V(t.h[:, 0:nch].rearrange("p (c o) -> p c o", o=1).to_broadcast([64, nch, 32]), t.trk)
            k.tt(v3(d1[i]), b3, bc(hbl[i]), ALU.subtract)
            k.act(Eq[i][:, 0:n], d1[i][:, 0:n], AF.Exp)
            k.act(Ek[i][:, 0:n], d1[i][:, 0:n], AF.Exp, scale=-1.0)
            k.act(Es[i][:, 0:n], b_[i][:, 0:n], AF.Exp)
            k.tt(v3(d1[i]), bc(bls[i]), b3, ALU.subtract)
            k.act(Ee[i][:, 0:n], d1[i][:, 0:n], AF.Exp)
            k.act(dec[i][:, 0:nch], bls[i][:, 0:nch], AF.Exp)
            k.tt(Eq[i][:, 0:n], Eq[i][:, 0:n], q_[i][:, 0:n], ALU.mult, en="pool")
            k.tt(Ek[i][:, 0:n], Ek[i][:, 0:n], kk[i][:, 0:n], ALU.mult, en="pool")
            k.tt(Es[i][:, 0:n], Es[i][:, 0:n], q_[i][:, 0:n], ALU.mult, en="pool")
            k.tt(Ee[i][:, 0:n], Ee[i][:, 0:n], kk[i][:, 0:n], ALU.mult)
            subs = range(nsub) if d == 0 else range(nsub - 1, -1, -1)
            for s in subs:
                j = scnt % 2; scnt += 1
                cs = s * 128; vt = (f0 + cs) // 128
                k.mm(ps_att[j][:, 0:128], Ek[i][:, cs:cs + 128], Eq[i][:, cs:cs + 128])
                k.tt(attm[j][:], ps_att[j][:, 0:128], masks[d][:], ALU.mult)
                k.transpose(ps_tr[:, 0:64], Ee[i][:, cs:cs + 128], ident[:])
                k.tt(ke_tm[j][:], V(ps_tr.h[:, 0:64].rearrange("p (o k) -> p o k", o=1).to_broadcast([128, 4, 64]), ps_tr.trk),
                     V(cm4.h[:].rearrange("p (c o) -> p c o", o=1).to_broadcast([128, 4, 64]), cm4.trk), ALU.mult)
                for c in range(4):
                    k.mm(ps_kv[j][:, c * 64:(c + 1) * 64], ke_tm[j][:, c, :], v_tm[:, vt, :])
                k.mm(ps_o[j][:, 0:128], v_tm[:, vt, :], attm[j][:], start=True, stop=False)
                chs = range(4) if d == 0 else range(3, -1, -1)
                for ci_, c in enumerate(chs):
                    S_cur = Sr.sub(slot % R, (slice(None), slot % R, slice(None)))
                    k.mm(ps_o[j][:, 32 * c:32 * c + 32], S_cur, Es[i][:, cs + 32 * c:cs + 32 * c + 32], start=False, stop=ci_ == 3)
                    S_nxt = Sr.sub((slot + 1) % R, (slice(None), (slot + 1) % R, slice(None)))
                    ch = s * 4 + c
                    k.stt(S_nxt, S_cur, dec[i][:, ch:ch + 1], ps_kv[j][:, c * 64:(c + 1) * 64], ALU.mult, ALU.add)
                    slot += 1
                if d == 0:
                    k.copy(Of[:, f0 + cs:f0 + cs + 128], ps_o[j][:, 0:128], en="act")
                else:
                    k.tt(osum[i][:, cs:cs + 128], Of[:, f0 + cs:f0 + cs + 128], ps_o[j][:, 0:128], ALU.add)
            if d == 1:
                k.tt(yt[i][:, 0:n], osum[i][:, 0:n], osum[i][:, 0:n], ALU.mult, en="pool")
                k.mm(ps_ss[:, 0:n], ones64[:], yt[i][:, 0:n])
                k.act(rstd[:, 0:n], ps_ss[:, 0:n], AF.Sqrt, bias=eps[:], scale=1.0 / 64)
                k.recip(rstd[:, 0:n], rstd[:, 0:n])
                k.tt(yt[i][:, 0:n], osum[i][:, 0:n], rstd[:, 0:n], ALU.mult)
                k.dma(ogt[i][:, 0:n], ogT[:, f0:f0 + n])
                k.act(sg[i][:, 0:n], ogt[i][:, 0:n], AF.Sigmoid)
                k.tt(sg[i][:, 0:n], sg[i][:, 0:n], ogt[i][:, 0:n], ALU.mult, en="pool")
                k.stt(yt[i][:, 0:n], yt[i][:, 0:n], ng[:, 0:1], sg[i][:, 0:n], ALU.mult, ALU.mult)
                k.dma(out[:, f0:f0 + n], yt[i][:, 0:n])
    k.finish(); k.close()
    return nc


def hgrn_maps(pl, pc, z, layer):
    maps = []
    for ci in range(NCORE):
        b, j = ci // 4, ci % 4
        full = np.concatenate([pc[b], pl[b]], 0)
        col = lambda base: np.ascontiguousarray(full[:, base + 64 * j: base + 64 * j + 64].T)
        hs = slice(64 * j, 64 * j + 64)
        maps.append({"qT": col(1280), "zfT": col(1536), "zbT": col(1792), "ogT": col(2304),
                     "v": np.ascontiguousarray(full[:, 2048 + 64 * j:2048 + 64 * j + 64]),
                     "lbl": np.ascontiguousarray(z['hgrn_lb_logits'][:, :, hs].transpose(2, 0, 1)),
                     "ng": np.ascontiguousarray(z['hgrn_norm_g'][layer][hs][:, None])})
    return maps


import numpy as np


def build_ffn(with_ctx, last, NE=32):
    nc = bass.Bass("TRN2", target_bir_lowering=False)
    k = K(nc)
    mixT = k.dram("mixT", [1024, TCORE]); xT = k.dram("xT", [1024, TCORE])
    w_out = k.dram("w_out", [1024, 1024]); g2d = k.dram("g2", [128, 8]); modv_d = k.dram("modv", [128, 8, 6, 2])
    rwd = k.dram("rw", [128, 8, 32]); rbd = k.dram("rb", [32, 1])
    wgu = k.dram("wgu", [32, 1024, 2048]); bgud = k.dram("bgu", [128, 32, 16])
    wdn = k.dram("wdn", [32, 1024, 1024]); bdnd = k.dram("bdn", [32, 1024])
    fgd = k.dram("fg", [128, 8])
    x1T = k.dram("x1T", [1024, TCORE], kind="ExternalOutput")
    outT = k.dram("outT", [1024, TCORE], kind="ExternalOutput")
    sb = k.sbuf
    modv = sb("modv_s", [128, 8, 6, 2]); k.dma(modv[:], modv_d[:, :, :, :])
    g2s = sb("g2s", [128, 8]); k.dma(g2s[:], g2d[:, :])
    fgs = sb("fgs", [128, 8]); k.dma(fgs[:], fgd[:, :])
    rw = sb("rw_s", [128, 8, 32]); k.dma(rw[:], rwd[:, :, :])
    rb = sb("rb_s", [32, 1]); k.dma(rb[:], rbd[:, :])
    bgu = sb("bgu_s", [128, 32, 16]); k.dma(bgu[:], bgud[:, :, :])
    bdn = sb("bdn_s", [32, 1024]); k.dma(bdn[:], bdnd[:, :])
    gs = setup_modscale(k, modv, g2s, 4, 3, "n2")
    ones = sb("ones", [128, 128]); k.memset(ones[:], 1.0)
    ident = ident_tile(k, "ident", 128)
    epsc = sb("epsc", [128, 1]); k.memset(epsc[:], 1e-6)
    NPMAX = 1088
    h2_bf = sb("h2_bf", [128, 8, NPMAX], BF16)
    acc = sb("acc", [128, 8, NPMAX])
    GT = sb("GT", [32, NPMAX])
    psm = k.psum("psm", [128, 512]); ps_ss = k.psum("ps_ss", [128, 512])
    psg = [k.psum("psg%d" % i, [128, 512]) for i in range(2)]
    psu = [k.psum("psu%d" % i, [128, 512]) for i in range(2)]
    psy = [k.psum("psy%d" % i, [128, 512]) for i in range(2)]
    xv = lambda t, c0, n: V(t.h[:, c0:c0 + n].rearrange("(k p) n -> p k n", p=128), t.trk)
    passes = []
    for p in range(4):
        tl = [(2 * p * 512, 512, 0), ((2 * p + 1) * 512, 512, 0)]
        if p == 0 and with_ctx:
            tl.append((4096, 64, 1))
        passes.append(tl)
    if not with_ctx:
        zt = sb("zt", [128, 8, 64]); k.memset(zt[:], 0.0)
        k.dma(xv(outT, 4096, 64), zt[:]); k.dma(xv(x1T, 4096, 64), zt[:])
    for pi, tl in enumerate(passes):
        offs = []; o = 0
        for (c0, n, grp) in tl:
            offs.append(o); o += n
        mk = k.mark()
        sb = lambda n_, s_, dt_=F32, pi=pi: k.sbuf("%s_p%d" % (n_, pi), s_, dt_)
        wk = norm_work(k, "n2p%d" % pi)
        wo_bf = sb("wo_bf", [128, 8, 1024], BF16); wst = sb("wo_st", [128, 1024])
        for kk in range(8):
            k.dma(wst[:], w_out[kk * 128:(kk + 1) * 128, :]); k.copy(wo_bf[:, kk, :], wst[:], en="pool")
        xt = sb("xt", [128, 8, 512]); mt = sb("mt", [128, 8, 512]); mbf = sb("mbf", [128, 8, 512], BF16)
        x1 = sb("x1", [128, 8, 512]); h2f = sb("h2f", [128, 8, 512]); LT = sb("LT", [32, 512])
        Ltm = sb("Ltm", [128, 32]); mx = sb("mx", [128, 8]); negm = sb("negm", [128, 1]); Ee = sb("Ee", [128, 32])
        Mm = sb("Mm", [128, 32]); ssum = sb("ssum", [128, 1]); Gt = sb("Gt", [128, 32])
        for ti, (c0, n, grp) in enumerate(tl):
            o = offs[ti]
            k.dma(mt[:, :, 0:n], xv(mixT, c0, n)); k.dma(xt[:, :, 0:n], xv(xT, c0, n))
            k.copy(mbf[:, :, 0:n], mt[:, :, 0:n], en="pool")
            for m in range(8):
                p = psy[m % 2]
                for kk in range(8):
                    k.mm(p[:, 0:n], wo_bf[:, kk, m * 128:(m + 1) * 128], mbf[:, kk, 0:n], start=kk == 0, stop=kk == 7)
                k.stt(x1[:, m, 0:n], p[:, 0:n], V(modv.h[:, m, 2, grp:grp + 1], modv.trk), xt[:, m, 0:n], ALU.mult, ALU.add)
            k.dma(xv(x1T, c0, n), x1[:, :, 0:n])
            hb = V(h2_bf.h[:, :, o:o + n], h2_bf.subs.setdefault(ti, Trk("h2bf%d" % ti)))
            class _HB:
                def __getitem__(s, idx):
                    return V(h2_bf.h[:, :, o:o + n][idx], hb.trk)
            emit_norm(k, x1, n, grp, gs, modv, 3, ones, ps_ss, wk, _HB(), h_f32=h2f)
            for kk in range(8):
                k.mm(psm[0:32, 0:n], rw[:, kk, :], h2f[:, kk, 0:n], start=kk == 0, stop=kk == 7)
            k.act(LT[:, 0:n], psm[0:32, 0:n], AF.Identity, bias=rb[:, 0:1])
            gtv = lambda a, b_: V(GT.h[:, a:b_], GT.subs.setdefault(ti, Trk("GT%d" % ti)))
            for s0 in range(0, n, 128):
                ns = min(128, n - s0)
                k.transpose(psy[0][0:ns, 0:32], LT[:, s0:s0 + ns], ident[0:32, 0:32])
                k.copy(Ltm[0:ns, :], psy[0][0:ns, 0:32])
                k.max8(mx[0:ns, :], Ltm[0:ns, :])
                k.ts(negm[0:ns, :], mx[0:ns, 0:1], -1.0, None, ALU.mult)
                k.act(Ee[0:ns, :], Ltm[0:ns, :], AF.Exp, bias=negm[0:ns, 0:1])
                k.ts(Mm[0:ns, :], Ltm[0:ns, :], mx[0:ns, 3:4], None, ALU.is_ge)
                k.tt(Ee[0:ns, :], Ee[0:ns, :], Mm[0:ns, :], ALU.mult)
                k.reduce(ssum[0:ns, :], Ee[0:ns, :])
                k.recip(ssum[0:ns, :], ssum[0:ns, :])
                k.ts(Gt[0:ns, :], Ee[0:ns, :], ssum[0:ns, 0:1], None, ALU.mult)
                k.transpose(psy[1][0:32, 0:ns], Gt[0:ns, :], ident[0:ns, 0:ns])
                k.copy(gtv(o + s0, o + s0 + ns), psy[1][0:32, 0:ns], en="act")
        k.release(mk)
        mk = k.mark()
        wgb = [sb("wgb%d" % i, [128, 8, 2048], BF16) for i in range(2)]
        wdb = [sb("wdb%d" % i, [128, 8, 1024], BF16) for i in range(2)]
        stg = [sb("stg%d" % i, [128, 1024]) for i in range(2)]
        actb = sb("actb", [128, 8, 512], BF16)
        G1 = [sb("G1%d" % i, [128, 512]) for i in range(2)]; SG = [sb("SG%d" % i, [128, 512]) for i in range(2)]
        U1 = [sb("U1%d" % i, [128, 512]) for i in range(2)]
        Gbc = sb("Gbc", [128, 512])
        accv = lambda ti, m, n: V(acc.h[:, m, offs[ti]:offs[ti] + n], acc.subs.setdefault(ti, Trk("acc%d" % ti)))
        for ti, (c0, n, grp) in enumerate(tl):
            k.memset(V(acc.h[:, :, offs[ti]:offs[ti] + n], acc.subs.setdefault(ti, Trk("acc%d" % ti))), 0.0, en="pool")
        sc = 0; fc = 0
        for e in range(NE):
            eb = e % 2
            for kk in range(8):
                for half in range(2):
                    s = stg[sc % 2]; sc += 1
                    k.dma(s[:], wgu[e, kk * 128:(kk + 1) * 128, half * 1024:(half + 1) * 1024])
                    k.copy(wgb[eb][:, kk, half * 1024:(half + 1) * 1024], s[:], en="pool")
            for kk in range(8):
                s = stg[sc % 2]; sc += 1
                k.dma(s[:], wdn[e, kk * 128:(kk + 1) * 128, :])
                k.copy(wdb[eb][:, kk, :], s[:], en="pool")
            for ti, (c0, n, grp) in enumerate(tl):
                o = offs[ti]
                hbv = lambda kk: V(h2_bf.h[:, kk, o:o + n], h2_bf.subs[ti])
                k.mm(psm[:, 0:n], V(ident.h[0:32, e:e + 1].to_broadcast([32, 128]), ident.trk), V(GT.h[:, o:o + n], GT.subs[ti]))
                k.copy(Gbc[:, 0:n], psm[:, 0:n], en="act")
                for f in range(8):
                    j = fc % 2; fc += 1
                    for kk in range(8):
                        k.mm(psg[j][:, 0:n], wgb[eb][:, kk, f * 128:(f + 1) * 128], hbv(kk), start=kk == 0, stop=kk == 7)
                    for kk in range(8):
                        k.mm(psu[j][:, 0:n], wgb[eb][:, kk, 1024 + f * 128:1024 + (f + 1) * 128], hbv(kk), start=kk == 0, stop=kk == 7)
                    k.ts(G1[j][:, 0:n], psg[j][:, 0:n], bgu[:, e, f:f + 1], 7.0, ALU.add, ALU.min)
                    k.act(SG[j][:, 0:n], G1[j][:, 0:n], AF.Sigmoid, scale=1.702)
                    k.tt(SG[j][:, 0:n], SG[j][:, 0:n], G1[j][:, 0:n], ALU.mult, en="pool")
                    k.ts(U1[j][:, 0:n], psu[j][:, 0:n], bgu[:, e, 8 + f:9 + f], 7.0, ALU.add, ALU.min)
                    k.ts(U1[j][:, 0:n], U1[j][:, 0:n], -7.0, 1.0, ALU.max, ALU.add)
                    k.tt(U1[j][:, 0:n], U1[j][:, 0:n], SG[j][:, 0:n], ALU.mult, en="pool")
                    k.tt(actb[:, f, 0:n], U1[j][:, 0:n], Gbc[:, 0:n], ALU.mult, en="pool")
                for m in range(8):
                    p = psy[m % 2]
                    for f in range(8):
                        k.mm(p[:, 0:n], wdb[eb][:, f, m * 128:(m + 1) * 128], actb[:, f, 0:n], start=f == 0, stop=f == 7)
                    k.tt(accv(ti, m, n), accv(ti, m, n), p[:, 0:n], ALU.add)
        k.release(mk)
        mk = k.mark()
        x1 = sb("x1c", [128, 8, 512]); x2 = sb("x2c", [128, 8, 512]); sq = sb("sqc", [128, 8, 512]); rs = sb("rsc", [128, 512])
        for ti, (c0, n, grp) in enumerate(tl):
            o = offs[ti]
            k.dma(x1[:, :, 0:n], xv(x1T, c0, n))
            for m in range(8):
                p = psy[m % 2]
                k.mm(p[:, 0:n], bdn[:, m * 128:(m + 1) * 128], V(GT.h[:, o:o + n], GT.subs[ti]))
                k.tt(x2[:, m, 0:n], accv(ti, m, n), p[:, 0:n], ALU.add)
                k.stt(x2[:, m, 0:n], x2[:, m, 0:n], V(modv.h[:, m, 5, grp:grp + 1], modv.trk), x1[:, m, 0:n], ALU.mult, ALU.add)
            if last:
                k.act(sq[:, :, 0:n], x2[:, :, 0:n], AF.Square)
                for kk in range(8):
                    k.mm(ps_ss[:, 0:n], ones[:], sq[:, kk, 0:n], start=kk == 0, stop=kk == 7)
                k.act(rs[:, 0:n], ps_ss[:, 0:n], AF.Sqrt, bias=epsc[:], scale=1.0 / 1024)
                k.recip(rs[:, 0:n], rs[:, 0:n])
                for m in range(8):
                    k.tt(x2[:, m, 0:n], x2[:, m, 0:n], rs[:, 0:n], ALU.mult)
                    k.ts(x2[:, m, 0:n], x2[:, m, 0:n], fgs[:, m:m + 1], None, ALU.mult)
            k.dma(xv(outT, c0, n), x2[:, :, 0:n])
        k.release(mk)
    k.finish(); k.close()
    print("ffn n_inst", k.n_inst)
    return nc


def ffn_maps(xl, xc, mix_l, mix_c, z, mod, layer):
    maps = []
    bgu = np.ascontiguousarray(z['moe_b_gu'][layer].reshape(32, 16, 128).transpose(2, 0, 1))
    rw = np.ascontiguousarray(z['router_w'][layer].reshape(8, 128, 32).transpose(1, 0, 2))
    for ci in range(NCORE):
        maps.append({"mixT": core_tokens_T(mix_l, mix_c, ci), "xT": core_tokens_T(xl, xc, ci), "w_out": z['w_out'][layer],
                     "g2": fm(z['norm2_g'][layer]), "modv": modv_for_core(mod, layer, ci // 4), "rw": rw,
                     "rb": np.ascontiguousarray(z['router_b'][layer][:, None]), "wgu": z['moe_w_gu'][layer], "bgu": bgu,
                     "wdn": z['moe_w_down'][layer], "bdn": z['moe_b_down'][layer], "fg": fm(z['final_g'])})
    return maps

def kernel(**inputs):
    z = {k_: np.ascontiguousarray(np.asarray(v_, dtype=np.float32)) for k_, v_ in inputs.items()}
    mod = host_adaln(z['c'], z['c_ctx'], z['ada_w'], z['ada_b'])
    xl, xc = z['x'], z['ctx']
    nc_in = build_inproj(); nc_at = build_attn(True); nc_lr = build_lru()
    for layer in range(2):
        pl, pc = host_inproj(nc_in, xl, xc, z['w_in'][layer], z['norm1_g'][layer], mod, layer)
        res = run(nc_at, attn_maps(pl, pc, z['attn_sink'][layer]))
        att = np.zeros((2, TF, 512), np.float32)
        for ci in range(NCORE):
            b, j = ci // 4, ci % 4
            att[b][:, j * 128:(j + 1) * 128] = res[ci]["attT"].transpose(2, 1, 0).reshape(TF, 128)
        res = run(nc_lr, lru_maps(pl, pc, z, layer))
        lru = np.zeros((2, TF, 256), np.float32)
        for ci in range(NCORE):
            b, j = ci // 4, ci % 4
            lru[b][:, j * 64:(j + 1) * 64] = res[ci]["lruT"].T
        res = run(build_hgrn(layer), hgrn_maps(pl, pc, z, layer))
        hg = np.zeros((2, TF, 256), np.float32)
        for ci in range(NCORE):
            b, j = ci // 4, ci % 4
            hg[b][:, j * 64:(j + 1) * 64] = res[ci]["hgT"].T
        mix = np.concatenate([att, lru, hg], -1)
        del pl, pc, att, lru, hg
        res = run(build_ffn(True, layer == 1), ffn_maps(xl, xc, mix[:, 256:], mix[:, :256], z, mod, layer))
        xl, xc = scatter_tokens([r["outT"] for r in res], 1024)
    return xl.astype(np.float32)
```
